# Optimizing a Trainium2 kernel written in Bass

```python
import math
import jax
import jax.numpy as jnp
from jax import lax
import numpy as np

D_MODEL = 2048
BATCH = 4
SEQ = 4096
DEPTH = 1

CHUNK = 64
Q_BLOCK = 128
DIFF_HEADS = 8
DIFF_HEAD_DIM = 64
DIFF_V_DIM = 2 * DIFF_HEAD_DIM
DSA_HEADS = 8
DSA_HEAD_DIM = 128
DSA_LATENT = 512
IDX_HEADS = 16
IDX_DIM = 64
DSA_TOPK_MAX = 256
MEM_TOKENS = 256
MEM_HEADS = 4
MEM_HEAD_DIM = 128
D_FF = 11 * D_MODEL // 4
BRANCH_A_WIDTH = DIFF_HEADS * DIFF_V_DIM
BRANCH_B_WIDTH = DSA_HEADS * DSA_HEAD_DIM
IN_WIDTHS = (DIFF_HEADS * 2 * DIFF_HEAD_DIM, DIFF_HEADS * 2 * DIFF_HEAD_DIM, DIFF_HEADS * DIFF_V_DIM,
             DSA_HEADS * DSA_HEAD_DIM, DSA_LATENT, IDX_HEADS * IDX_DIM, IDX_DIM, IDX_HEADS)
IN_TOTAL = sum(IN_WIDTHS)
ALPHA = (2.0 * DEPTH) ** 0.25
BETA = (8.0 * DEPTH) ** -0.25
LN_EPS = 1e-5

kernel_name = 'hybrid_diff_dsa_macaron_deepnorm_layer'


def _layer_norm(x, g, b):
    xf = x.astype(jnp.float32)
    mu = jnp.mean(xf, axis=-1, keepdims=True)
    var = jnp.mean(jnp.square(xf - mu), axis=-1, keepdims=True)
    return ((xf - mu) * lax.rsqrt(var + LN_EPS) * g.astype(jnp.float32) + b.astype(jnp.float32)).astype(x.dtype)


def _rms_norm(x, g):
    xf = x.astype(jnp.float32)
    return (xf * lax.rsqrt(jnp.mean(jnp.square(xf), axis=-1, keepdims=True) + LN_EPS) * g.astype(jnp.float32)).astype(x.dtype)


def _alibi_slopes(n):
    return 2.0 ** (-8.0 * jnp.arange(1, n + 1, dtype=jnp.float32) / n)


def _swiglu(x, w_gate, w_up, w_down):
    return (jax.nn.silu(x @ w_gate) * (x @ w_up)) @ w_down


def _chunk_admissible(q_pos, k_pos):
    return (k_pos // CHUNK)[None, :] <= (q_pos // CHUNK)[:, None]


def _diff_attention(q, k, v, lam, sub_g, lambda_init):
    seq = q.shape[1]
    slopes = _alibi_slopes(DIFF_HEADS)
    scale = DIFF_HEAD_DIM ** -0.5
    pos = jnp.arange(seq, dtype=jnp.int32)
    outs = []
    for start in range(0, seq, Q_BLOCK):
        end = start + Q_BLOCK
        q_pos, k_pos = pos[start:end], pos[:end]
        logits = jnp.einsum('bthcd,bshcd->bhcts', q[:, start:end], k[:, :end]).astype(jnp.float32) * scale
        dist = jnp.abs(q_pos[:, None] - k_pos[None, :]).astype(jnp.float32)
        logits = logits - (slopes[:, None, None] * dist)[None, :, None]
        logits = jnp.where(_chunk_admissible(q_pos, k_pos), logits, -jnp.inf)
        p = jax.nn.softmax(logits, axis=-1)
        attn = p[:, :, 0] - lam * p[:, :, 1]
        outs.append(jnp.einsum('bhts,bshe->bthe', attn.astype(v.dtype), v[:, :end]))
    o = jnp.concatenate(outs, axis=1)
    o = _rms_norm(o, sub_g) * (1.0 - lambda_init)
    return o.reshape(o.shape[0], seq, BRANCH_A_WIDTH)


def _dsa_attention(q, c, iq, ik, iw, w_uk, w_uv, kv_g):
    batch, seq = q.shape[0], q.shape[1]
    top_k = min(DSA_TOPK_MAX, seq // 4)
    slopes = _alibi_slopes(DSA_HEADS)
    scale = DSA_HEAD_DIM ** -0.5
    idx_scale = (IDX_HEADS * IDX_DIM) ** -0.5
    c = _rms_norm(c, kv_g)
    q_lat = jnp.einsum('bthd,hcd->bthc', q, w_uk)
    k_pos = jnp.arange(seq, dtype=jnp.int32)
    gather = jax.vmap(lambda cb, ib: cb[ib])

    def one_block(start):
        q_pos = start + jnp.arange(Q_BLOCK, dtype=jnp.int32)
        iq_b = lax.dynamic_slice_in_dim(iq, start, Q_BLOCK, axis=1)
        iw_b = lax.dynamic_slice_in_dim(iw, start, Q_BLOCK, axis=1)
        ql_b = lax.dynamic_slice_in_dim(q_lat, start, Q_BLOCK, axis=1)
        head_scores = jax.nn.relu(jnp.einsum('bthd,bsd->bths', iq_b, ik))
        score = jnp.einsum('bths,bth->bts', head_scores, iw_b).astype(jnp.float32) * idx_scale
        score = jnp.where(_chunk_admissible(q_pos, k_pos)[None], score, -jnp.inf)
        _, sel = lax.top_k(score, top_k)
        c_sel = gather(c, sel)
        valid = (sel // CHUNK) <= (q_pos // CHUNK)[None, :, None]
        dist = jnp.abs(q_pos[None, :, None] - sel).astype(jnp.float32)
        logits = jnp.einsum('bthc,btkc->bthk', ql_b, c_sel).astype(jnp.float32) * scale
        logits = logits - slopes[None, None, :, None] * dist[:, :, None, :]
        logits = jnp.where(valid[:, :, None, :], logits, -jnp.inf)
        p = jax.nn.softmax(logits, axis=-1).astype(c.dtype)
        o_lat = jnp.einsum('bthk,btkc->bthc', p, c_sel)
        return jnp.einsum('bthc,hcd->bthd', o_lat, w_uv)

    starts = jnp.arange(0, seq, Q_BLOCK, dtype=jnp.int32)
    o = lax.map(one_block, starts)
    return jnp.moveaxis(o, 0, 1).reshape(batch, seq, BRANCH_B_WIDTH)


def _hybrid_mixer(x, w_in, diff_lambda, diff_subln_g, lambda_init, dsa_kv_g, dsa_w_uk, dsa_w_uv,
                  w_gate, b_gate, w_branch_a, w_branch_b, w_mix_out):
    b, s = x.shape[0], x.shape[1]
    h = x @ w_in
    aq, ak, av, bq, bc, iq, ik, iw = jnp.split(h, np.cumsum(IN_WIDTHS)[:-1].tolist(), axis=-1)
    lf = diff_lambda.astype(jnp.float32)
    lam = jnp.exp(jnp.sum(lf[0] * lf[1])) - jnp.exp(jnp.sum(lf[2] * lf[3])) + lambda_init
    y_a = _diff_attention(aq.reshape(b, s, DIFF_HEADS, 2, DIFF_HEAD_DIM),
                          ak.reshape(b, s, DIFF_HEADS, 2, DIFF_HEAD_DIM),
                          av.reshape(b, s, DIFF_HEADS, DIFF_V_DIM), lam, diff_subln_g, lambda_init)
    y_b = _dsa_attention(bq.reshape(b, s, DSA_HEADS, DSA_HEAD_DIM), bc,
                         iq.reshape(b, s, IDX_HEADS, IDX_DIM), ik, iw, dsa_w_uk, dsa_w_uv, dsa_kv_g)
    gates = jax.nn.sigmoid(jnp.einsum('bsd,gde->gbse', x, w_gate) + b_gate[:, None, None, :])
    merged = gates[0] * (y_a @ w_branch_a) + gates[1] * (y_b @ w_branch_b)
    return merged @ w_mix_out


def _memory_attention(x, mem, w_q, w_kv, w_o):
    b, s = x.shape[0], x.shape[1]
    q = (x @ w_q).reshape(b, s, MEM_HEADS, MEM_HEAD_DIM)
    kv = (mem @ w_kv).reshape(b, mem.shape[1], 2, MEM_HEADS, MEM_HEAD_DIM)
    logits = jnp.einsum('bthd,bmhd->bhtm', q, kv[:, :, 0]).astype(jnp.float32) * MEM_HEAD_DIM ** -0.5
    p = jax.nn.softmax(logits, axis=-1).astype(x.dtype)
    o = jnp.einsum('bhtm,bmhd->bthd', p, kv[:, :, 1])
    return o.reshape(b, s, MEM_HEADS * MEM_HEAD_DIM) @ w_o


def setup_inputs(seed: int = 0) -> dict:
    key = jax.random.key(seed)
    ks = iter(jax.random.split(key, 32))
    f32 = jnp.float32
    L = DEPTH

    def w(shape, fan_in, gain=1.0):
        return jax.random.normal(next(ks), shape, f32) * (gain * fan_in ** -0.5)

    def noise(shape, s):
        return jax.random.normal(next(ks), shape, f32) * s

    return {
        'x': jax.random.normal(next(ks), (BATCH, SEQ, D_MODEL), f32),
        'mem': jax.random.normal(next(ks), (BATCH, MEM_TOKENS, D_MODEL), f32),
        'ffn1_w_gate': w((L, D_MODEL, D_FF), D_MODEL),
        'ffn1_w_up': w((L, D_MODEL, D_FF), D_MODEL),
        'ffn1_w_down': w((L, D_FF, D_MODEL), D_FF, BETA),
        'w_in': w((L, D_MODEL, IN_TOTAL), D_MODEL),
        'diff_lambda': noise((L, 4, DIFF_HEAD_DIM), 0.1),
        'diff_subln_g': 1.0 + noise((L, DIFF_V_DIM), 0.02),
        'dsa_kv_g': 1.0 + noise((L, DSA_LATENT), 0.02),
        'dsa_w_uk': w((L, DSA_HEADS, DSA_LATENT, DSA_HEAD_DIM), DSA_LATENT),
        'dsa_w_uv': w((L, DSA_HEADS, DSA_LATENT, DSA_HEAD_DIM), DSA_LATENT),
        'w_gate': w((L, 2, D_MODEL, D_MODEL), D_MODEL),
        'b_gate': noise((L, 2, D_MODEL), 0.02),
        'w_branch_a': w((L, BRANCH_A_WIDTH, D_MODEL), BRANCH_A_WIDTH),
        'w_branch_b': w((L, BRANCH_B_WIDTH, D_MODEL), BRANCH_B_WIDTH),
        'w_mix_out': w((L, D_MODEL, D_MODEL), D_MODEL, BETA),
        'mem_w_q': w((L, D_MODEL, MEM_HEADS * MEM_HEAD_DIM), D_MODEL),
        'mem_w_kv': w((L, D_MODEL, 2 * MEM_HEADS * MEM_HEAD_DIM), D_MODEL),
        'mem_w_o': w((L, MEM_HEADS * MEM_HEAD_DIM, D_MODEL), MEM_HEADS * MEM_HEAD_DIM, BETA),
        'ffn2_w_gate': w((L, D_MODEL, D_FF), D_MODEL),
        'ffn2_w_up': w((L, D_MODEL, D_FF), D_MODEL),
        'ffn2_w_down': w((L, D_FF, D_MODEL), D_FF, BETA),
        'ln_g': 1.0 + noise((L, 4, D_MODEL), 0.02),
        'ln_b': noise((L, 4, D_MODEL), 0.02),
    }


def reference(x, mem, ffn1_w_gate, ffn1_w_up, ffn1_w_down, w_in, diff_lambda, diff_subln_g,
              dsa_kv_g, dsa_w_uk, dsa_w_uv, w_gate, b_gate, w_branch_a, w_branch_b, w_mix_out,
              mem_w_q, mem_w_kv, mem_w_o, ffn2_w_gate, ffn2_w_up, ffn2_w_down, ln_g, ln_b):
    for l in range(DEPTH):
        lambda_init = 0.8 - 0.6 * math.exp(-0.3 * l)
        x = _layer_norm(ALPHA * x + 0.5 * _swiglu(x, ffn1_w_gate[l], ffn1_w_up[l], ffn1_w_down[l]),
                        ln_g[l, 0], ln_b[l, 0])
        x = _layer_norm(ALPHA * x + _hybrid_mixer(x, w_in[l], diff_lambda[l], diff_subln_g[l], lambda_init,
                                                  dsa_kv_g[l], dsa_w_uk[l], dsa_w_uv[l], w_gate[l], b_gate[l],
                                                  w_branch_a[l], w_branch_b[l], w_mix_out[l]),
                        ln_g[l, 1], ln_b[l, 1])
        x = _layer_norm(ALPHA * x + _memory_attention(x, mem, mem_w_q[l], mem_w_kv[l], mem_w_o[l]),
                        ln_g[l, 2], ln_b[l, 2])
        x = _layer_norm(ALPHA * x + 0.5 * _swiglu(x, ffn2_w_gate[l], ffn2_w_up[l], ffn2_w_down[l]),
                        ln_g[l, 3], ln_b[l, 3])
    return x
```

```python
from contextlib import ExitStack
import numpy as np
import os
SKIP = os.environ.get('KSKIP', '')
import concourse.bass as bass
import concourse.mybir as mybir
from concourse.bass_utils import run_bass_kernel_spmd

F32 = mybir.dt.float32
BF16 = mybir.dt.bfloat16
ALU = mybir.AluOpType
AF = mybir.ActivationFunctionType
AX = mybir.AxisListType

D = 2048
DC = 16
SEQ = 4096
NTOK = 4096
NQ = 2048
DFF = 5632
FC = 44
T = 512
IN_TOTAL = 5712
ALPHA = 2.0 ** 0.25
LN_EPS = 1e-5
LAMBDA_INIT = 0.2
NEG = -30000.0
EPOCH = 20000


class Prog:
    ENGS = ("pe", "act", "dve", "pool", "sp")

    def __init__(self, nc, stack):
        self.nc = nc
        self.stack = stack
        self.csems = {e: [] for e in self.ENGS}
        self.ctargets = {e: 0 for e in self.ENGS}
        self.dsems = {}
        self.dcount = {}
        self.reset_phase()

    def reset_phase(self):
        self.ins = {e: [] for e in self.ENGS}
        self.res = {}
        self.waited = {e: {} for e in self.ENGS}
        self.pending_dma = {e: {} for e in self.ENGS}

    def _need(self, eng, tok, waits):
        if tok is None:
            return
        if tok[0] == "c":
            if tok[1] == eng and eng == "pe":
                return
            key = ("c", tok[1])
        else:
            key = ("d", tok[1])
        if self.waited[eng].get(key, -1) >= tok[2]:
            return
        self.waited[eng][key] = tok[2]
        waits.append(tok)

    def op(self, eng, fn, reads=(), writes=(), dma=None):
        waits = []
        for k in reads:
            r = self.res.get(k)
            if r is not None:
                self._need(eng, r["w"], waits)
        for k in writes:
            r = self.res.get(k)
            if r is not None:
                self._need(eng, r["w"], waits)
                for t in r["r"].values():
                    self._need(eng, t, waits)
        idx = len(self.ins[eng])
        if dma is None:
            tok = ("c", eng, idx)
        else:
            self.dcount[dma] = self.dcount.get(dma, 0) + 1
            tok = ("d", dma, self.dcount[dma])
            self.pending_dma[eng][dma] = tok
        rec = {"fn": fn, "waits": waits, "tok": tok, "target": False}
        self.ins[eng].append(rec)
        for k in reads:
            r = self.res.setdefault(k, {"w": None, "r": {}})
            r["r"][(tok[0], tok[1])] = tok
        for k in writes:
            self.res[k] = {"w": tok, "r": {}}
        return tok

    def _csem(self, eng, rank):
        ep = (rank - 1) // EPOCH
        while len(self.csems[eng]) <= ep:
            self.csems[eng].append(self.stack.enter_context(self.nc.semaphore(f"c_{eng}_{len(self.csems[eng])}")))
        return self.csems[eng][ep], (rank - 1) % EPOCH + 1

    def _dsem(self, stream, k):
        ep = (k - 1) // 1000
        lst = self.dsems.setdefault(stream, [])
        while len(lst) <= ep:
            lst.append(self.stack.enter_context(self.nc.semaphore(f"d_{stream}_{len(lst)}")))
        return lst[ep], 16 * ((k - 1) % 1000 + 1)

    def emit(self):
        nc = self.nc
        for e in self.ENGS:
            for tok in list(self.pending_dma[e].values()):
                w = []
                self._need(e, tok, w)
                if w:
                    self.ins[e].append({"fn": None, "waits": w, "tok": None, "target": False})
        for e in self.ENGS:
            for rec in self.ins[e]:
                for t in rec["waits"]:
                    if t[0] == "c":
                        self.ins[t[1]][t[2]]["target"] = True
        rank = {}
        for e in self.ENGS:
            for i, rec in enumerate(self.ins[e]):
                if rec["target"]:
                    self.ctargets[e] += 1
                    rank[(e, i)] = self.ctargets[e]
        engobj = {"pe": "tensor", "act": "scalar", "dve": "vector", "pool": "gpsimd", "sp": "sync"}
        with nc.Block() as block:
            for e in self.ENGS:
                recs = self.ins[e]
                if not recs:
                    continue

                def body(eng, recs=recs, e=e):
                    for i, rec in enumerate(recs):
                        for t in rec["waits"]:
                            if t[0] == "c":
                                sem, val = self._csem(t[1], rank[(t[1], t[2])])
                            else:
                                sem, val = self._dsem(t[1], t[2])
                            eng.wait_ge(sem, val)
                        if rec["fn"] is None:
                            continue
                        ins = rec["fn"](eng)
                        tok = rec["tok"]
                        if tok[0] == "d":
                            sem, val = self._dsem(tok[1], tok[2])
                            ins.then_inc(sem, 16)
                        elif rec["target"]:
                            sem, val = self._csem(e, rank[(e, i)])
                            ins.then_inc(sem, 1)

                getattr(block, engobj[e])(body)
        self.reset_phase()


def _build(nc, dbg=None):
    _n = [0]

    def U(name):
        _n[0] += 1
        return f"{name}_u{_n[0]}"
    st = ExitStack()
    with st:
        P = Prog(nc, st)

        def din(name, shape, dt=F32):
            return nc.dram_tensor(name, list(shape), dt, kind="ExternalInput").ap()

        def dscr(name, shape, dt):
            return nc.dram_tensor(name, list(shape), dt, kind="Internal").ap()

        xl = din("xl", [NTOK, D])
        memb = din("memb", [256, D])
        par = din("par", [128, 1])
        btab_d = din("btab", [128, 8 * 32])
        bdiag_d = din("bdiag", [128, 8 * 128])
        mdiag_d = din("mdiag", [128, 128])
        negr_d = din("negr", [128, 256])
        dtab_d = din("dtab", [128, 32])
        W = {}
        for name, shape in [
            ("ffn1_w_gate", [D, DFF]), ("ffn1_w_up", [D, DFF]), ("ffn1_w_down", [DFF, D]),
            ("w_in", [D, IN_TOTAL]), ("diff_lambda", [4, 64]), ("diff_subln_g", [1, 128]),
            ("dsa_kv_g", [1, 512]), ("dsa_w_uk", [8, 512, 128]), ("dsa_w_uv", [8, 512, 128]),
            ("w_gate", [2, D, D]), ("b_gate", [2, D]), ("w_branch_a", [1024, D]), ("w_branch_b", [1024, D]),
            ("w_mix_out", [D, D]), ("mem_w_q", [D, 512]), ("mem_w_kv", [D, 1024]), ("mem_w_o", [512, D]),
            ("ffn2_w_gate", [D, DFF]), ("ffn2_w_up", [D, DFF]), ("ffn2_w_down", [DFF, D]),
            ("ln_g", [4, D]), ("ln_b", [4, D]),
        ]:
            W[name] = din(name, shape)
        out = nc.dram_tensor("out", [NQ, D], F32, kind="ExternalOutput").ap()

        x1T = dscr("x1T", [D, NTOK], BF16)
        x1f = dscr("x1f", [D, NQ], F32)
        if dbg is not None:
            dbg_out = nc.dram_tensor("dbg", list(dbg[1]), F32, kind="ExternalOutput").ap()

        ones32 = st.enter_context(nc.sbuf_tensor("ones32", [128, 128], F32))
        ident32 = st.enter_context(nc.sbuf_tensor("ident32", [128, 128], F32))
        lng = st.enter_context(nc.sbuf_tensor("lng", [128, 4, DC], F32))
        lnb = st.enter_context(nc.sbuf_tensor("lnb", [128, 4, DC], F32))

        def setup_consts():
            P.op("pool", lambda e: e.memset(ones32[:], 1.0), writes=["ones32"])
            P.op("pool", lambda e: e.memset(ident32[:], 0.0), writes=["ident32"])
            P.op("pool", lambda e: e.affine_select(out=ident32[:], in_=ident32[:], pattern=[[-1, 128]],
                                                   compare_op=ALU.not_equal, fill=1.0, base=0,
                                                   channel_multiplier=1),
                 reads=["ident32"], writes=["ident32"])
            with nc.sbuf_tensor("lnraw", [128, 128], F32) as lnraw, nc.psum_tensor("pst0", [128, 512], F32) as pst0:
                P.op("sp", lambda e: e.dma_start(out=lnraw[0:64, :], in_=W["ln_g"].rearrange("l (c p) -> (l c) p", p=128)),
                     writes=["lnraw0"], dma="cg")
                P.op("sp", lambda e: e.dma_start(out=lnraw[64:128, :], in_=W["ln_b"].rearrange("l (c p) -> (l c) p", p=128)),
                     writes=["lnraw1"], dma="cb")
                P.op("pe", lambda e: e.transpose(pst0[:, 0:128], lnraw[:, :], ident32[:]),
                     reads=["lnraw0", "lnraw1", "ident32"], writes=["pst0"])
                P.op("dve", lambda e: e.tensor_copy(out=lng[:].rearrange("p l c -> p (l c)"), in_=pst0[:, 0:64]),
                     reads=["pst0"], writes=[("lng", l) for l in range(4)])
                P.op("dve", lambda e: e.tensor_copy(out=lnb[:].rearrange("p l c -> p (l c)"), in_=pst0[:, 64:128]),
                     reads=["pst0"], writes=[("lnb", l) for l in range(4)])
                P.emit()

        class WStream:
            def __init__(self, slots):
                self.slots = slots
                self.n = 0

            def load(self, Wap, k0, kg, f0, ncols):
                s = self.n % len(self.slots)
                self.n += 1
                slot = self.slots[s]
                view = slot[:, 0:kg * ncols].rearrange("p (k f) -> p k f", f=ncols)
                half = (kg + 1) // 2
                for (a, b) in ((0, half), (half, kg)):
                    if b <= a:
                        continue
                    src = Wap[(k0 + a) * 128:(k0 + b) * 128, f0:f0 + ncols].rearrange("(k p) f -> p k f", p=128)
                    P.op("pool", lambda e, a=a, b=b, src=src: e.dma_start(out=view[:, a:b, :], in_=src),
                         writes=[("ws", s)] if a == 0 else [], reads=[] if a == 0 else [], dma=f"ws{s}")
                P.res[("ws", s)]["w"] = ("d", f"ws{s}", P.dcount[f"ws{s}"])
                return view, ("ws", s)

        def linear(ws, Wap, KC, f0, F, xT, xkey, Tn, psum, pskeys, evac, ncols=256):
            nb = len(psum)
            gi = 0
            for g0 in range(0, F, ncols):
                nc_ = min(ncols, F - g0)
                nj = (nc_ + 127) // 128
                banks = [(gi * nj + j) % nb for j in range(nj)]
                gi += 1
                for k0 in range(0, KC, 16):
                    kg = min(16, KC - k0)
                    view, wkey = ws.load(Wap, k0, kg, f0 + g0, nc_)
                    for j in range(nj):
                        m = min(128, nc_ - j * 128)
                        for k in range(kg):
                            kc = k0 + k
                            P.op("pe", lambda e, j=j, k=k, kc=kc, m=m, view=view, bk=banks[j]: e.matmul(
                                psum[bk][0:m, 0:Tn], lhsT=view[:, k, j * 128:j * 128 + m], rhs=xT(kc),
                                start=(kc == 0), stop=(kc == KC - 1)),
                                reads=[wkey, xkey(kc)], writes=[pskeys[banks[j]]])
                for j in range(nj):
                    evac((g0 // 128) + j, psum[banks[j]], pskeys[banks[j]])

        def layer_norm(z, zkey, s1, s2, ps_a, ps_a_key, ps_b, ps_b_key, tmp, l, outs, Tn=T):
            eps = LN_EPS / (ALPHA * ALPHA)
            P.op("pe", lambda e: e.matmul(ps_a[:, 0:Tn], lhsT=ones32[:], rhs=s1[:, 0:Tn], start=True, stop=True),
                 reads=["ones32", "s1"], writes=[ps_a_key])
            P.op("pe", lambda e: e.matmul(ps_b[:, 0:Tn], lhsT=ones32[:], rhs=s2[:, 0:Tn], start=True, stop=True),
                 reads=["ones32", "s2"], writes=[ps_b_key])
            mean, rstd, t2 = tmp
            P.op("dve", lambda e: e.tensor_scalar(out=mean[:, 0:Tn], in0=ps_a[:, 0:Tn], scalar1=1.0 / D, scalar2=None,
                                                  op0=ALU.mult), reads=[ps_a_key], writes=["ln_mean"])
            P.op("dve", lambda e: e.tensor_tensor(out=t2[:, 0:Tn], in0=mean[:, 0:Tn], in1=mean[:, 0:Tn], op=ALU.mult),
                 reads=["ln_mean"], writes=["ln_t2"])
            P.op("dve", lambda e: e.scalar_tensor_tensor(out=t2[:, 0:Tn], in0=ps_b[:, 0:Tn], scalar=1.0 / D, in1=t2[:, 0:Tn],
                                                         op0=ALU.mult, op1=ALU.subtract),
                 reads=[ps_b_key, "ln_t2"], writes=["ln_t2"])
            P.op("dve", lambda e: e.tensor_scalar(out=t2[:, 0:Tn], in0=t2[:, 0:Tn], scalar1=eps, scalar2=None, op0=ALU.add),
                 reads=["ln_t2"], writes=["ln_t2"])
            P.op("act", lambda e: e.activation(out=t2[:, 0:Tn], in_=t2[:, 0:Tn], func=AF.Sqrt), reads=["ln_t2"], writes=["ln_t2"])
            P.op("dve", lambda e: e.reciprocal(out=rstd[:, 0:Tn], in_=t2[:, 0:Tn]), reads=["ln_t2"], writes=["ln_rstd"])
            for c in range(DC):
                P.op("dve", lambda e, c=c: e.tensor_tensor(out=z[:, c, 0:Tn], in0=z[:, c, 0:Tn], in1=mean[:, 0:Tn], op=ALU.subtract),
                     reads=[zkey(c), "ln_mean"], writes=[zkey(c)])
                P.op("dve", lambda e, c=c: e.tensor_tensor(out=z[:, c, 0:Tn], in0=z[:, c, 0:Tn], in1=rstd[:, 0:Tn], op=ALU.mult),
                     reads=[zkey(c), "ln_rstd"], writes=[zkey(c)])
                for (ofn, okey) in outs:
                    P.op("act", lambda e, c=c, ofn=ofn: e.activation(out=ofn(c), in_=z[:, c, 0:Tn], func=AF.Identity,
                                                                      bias=lnb[:, l, c:c + 1], scale=lng[:, l, c:c + 1]),
                         reads=[zkey(c), ("lng", l), ("lnb", l)], writes=[okey(c)])

        def ffn_phase(ntiles, load_xT, wg, wu, wd, l, store):
            with ExitStack() as ps:
                sb = lambda name, shape, dt: ps.enter_context(nc.sbuf_tensor(U(name), shape, dt))
                xT32 = sb("xT32", [128, DC, T], F32)
                xTb = sb("xTb", [128, DC, T], BF16)
                hT = sb("hT", [128, FC, T], BF16)
                wsl = [sb(f"ws{i}", [128, 16 * 256], BF16) for i in range(4)]
                sg = [sb(f"sg{i}", [128, T], BF16) for i in range(4)]
                s1 = sb("s1", [128, T], F32)
                s2 = sb("s2", [128, T], F32)
                sq = [sb(f"sq{i}", [128, T], F32) for i in range(2)]
                mean = sb("mean", [128, T], F32)
                rstd = sb("rstd", [128, T], F32)
                t2 = sb("t2", [128, T], F32)
                xtok = [sb(f"xtok{i}", [128, D], F32) for i in range(2)]
                ob = sb("ob", [128, DC, T], BF16)
                psum = [ps.enter_context(nc.psum_tensor(U(f"ps{i}"), [128, 512], F32)) for i in range(8)]
                pskeys = [("ps", i) for i in range(8)]
                ws = WStream(wsl)
                env = dict(xT32=xT32, xTb=xTb, xtok=xtok, psum=psum, pskeys=pskeys, ob=ob)
                for tt in range(ntiles):
                    if 'ld' not in SKIP:
                        load_xT(tt, env)
                    for g0 in range(0, 0 if 'gu' in SKIP else DFF, 256):
                        gb = ((g0 // 256) % 2) * 4
                        cnt = {"n": 0}

                        def evac_g(j, pt, pk, gb=gb):
                            jj = j % 2
                            P.op("act", lambda e: e.activation(out=sg[gb // 2 + jj][:], in_=pt[:, 0:T], func=AF.Silu),
                                 reads=[pk], writes=[("sg", gb // 2 + jj)])

                        def evac_u(j, pt, pk, gb=gb, g0=g0):
                            jj = j % 2
                            jg = g0 // 128 + j
                            P.op("dve", lambda e: e.tensor_tensor(out=hT[:, jg, :], in0=pt[:, 0:T], in1=sg[gb // 2 + jj][:], op=ALU.mult),
                                 reads=[pk, ("sg", gb // 2 + jj)], writes=[("hT", jg)])

                        linear(ws, wg, DC, g0, 256, lambda kc: xTb[:, kc, :], lambda kc: ("xTb", kc), T,
                               psum[gb:gb + 2], pskeys[gb:gb + 2], evac_g)
                        linear(ws, wu, DC, g0, 256, lambda kc: xTb[:, kc, :], lambda kc: ("xTb", kc), T,
                               psum[gb + 2:gb + 4], pskeys[gb + 2:gb + 4], evac_u)
                    def evac_d(j, pt, pk):
                        P.op("dve", lambda e: e.scalar_tensor_tensor(out=xT32[:, j, :], in0=pt[:, 0:T], scalar=0.5 / ALPHA,
                                                                     in1=xT32[:, j, :], op0=ALU.mult, op1=ALU.add),
                             reads=[pk, ("xT32", j)], writes=[("xT32", j)])
                        q = sq[j % 2]
                        P.op("act", lambda e: e.activation(out=q[:], in_=xT32[:, j, :], func=AF.Square),
                             reads=[("xT32", j)], writes=[("sq", j % 2)])
                        if j == 0:
                            P.op("dve", lambda e: e.tensor_copy(out=s1[:], in_=xT32[:, j, :]), reads=[("xT32", j)], writes=["s1"])
                            P.op("dve", lambda e: e.tensor_copy(out=s2[:], in_=q[:]), reads=[("sq", j % 2)], writes=["s2"])
                        else:
                            P.op("dve", lambda e: e.tensor_tensor(out=s1[:], in0=s1[:], in1=xT32[:, j, :], op=ALU.add),
                                 reads=[("xT32", j), "s1"], writes=["s1"])
                            P.op("dve", lambda e: e.tensor_tensor(out=s2[:], in0=s2[:], in1=q[:], op=ALU.add),
                                 reads=[("sq", j % 2), "s2"], writes=["s2"])

                    linear(ws, wd, 2 if 'dn' in SKIP else FC, 0, D, lambda kc: hT[:, kc, :], lambda kc: ("hT", kc), T,
                           psum[0:4], pskeys[0:4], evac_d)
                    outs = store(tt, env)
                    if 'ln' not in SKIP:
                        layer_norm(xT32, lambda c: ("xT32", c), s1, s2, psum[4], pskeys[4], psum[5], pskeys[5],
                                   (mean, rstd, t2), l, outs["ln_outs"])
                    outs["after"]()
                P.emit()

        def load_x_tile(tt, env):
            xT32, xTb, xtok, psum, pskeys = env["xT32"], env["xTb"], env["xtok"], env["psum"], env["pskeys"]
            for sub in range(4):
                r0 = tt * T + sub * 128
                xk = xtok[sub % 2]
                P.op("sp", lambda e, r0=r0, xk=xk: e.dma_start(out=xk[:], in_=xl[r0:r0 + 128, :]),
                     writes=[("xtok", sub % 2)], dma=f"xtok{sub % 2}")
                for c4 in range(4):
                    bk = 6 + (c4 % 2)
                    for cc in range(4):
                        c = c4 * 4 + cc
                        P.op("pe", lambda e, c=c, cc=cc, bk=bk, xk=xk: e.transpose(
                            psum[bk][:, cc * 128:(cc + 1) * 128], xk[:, c * 128:(c + 1) * 128], ident32[:]),
                            reads=[("xtok", sub % 2), "ident32"], writes=[pskeys[bk]])
                    for cc in range(4):
                        c = c4 * 4 + cc
                        P.op("act", lambda e, c=c, cc=cc, sub=sub, bk=bk: e.copy(out=xT32[:, c, sub * 128:(sub + 1) * 128],
                                                                                in_=psum[bk][:, cc * 128:(cc + 1) * 128]),
                             reads=[pskeys[bk]], writes=[("xT32", c)])
                        P.op("dve", lambda e, c=c, sub=sub: e.tensor_copy(out=xTb[:, c, sub * 128:(sub + 1) * 128],
                                                                          in_=xT32[:, c, sub * 128:(sub + 1) * 128]),
                             reads=[("xT32", c)], writes=[("xTb", c)])

        def store_a(tt, env):
            ob, xT32 = env["ob"], env["xT32"]
            t0 = tt * T
            ln_outs = [(lambda c: ob[:, c, :], lambda c: ("ob", c))]
            if tt < NQ // T:
                ln_outs.append((lambda c: xT32[:, c, :], lambda c: ("xT32", c)))

            def after():
                P.op("sp", lambda e: e.dma_start(out=x1T[:, t0:t0 + T].rearrange("(c p) t -> p c t", p=128), in_=ob[:]),
                     reads=[("ob", c) for c in range(DC)], dma="st_ob")
                if tt < NQ // T:
                    P.op("sp", lambda e: e.dma_start(out=x1f[:, t0:t0 + T].rearrange("(c p) t -> p c t", p=128), in_=xT32[:]),
                         reads=[("xT32", c) for c in range(DC)], dma="st_x32")
            return {"ln_outs": ln_outs, "after": after}

        setup_consts()
        STOP = os.environ.get("KSTOP", "")
        NTA = int(os.environ.get("KNTA", NTOK // T))
        ffn_phase(NTA, load_x_tile, W["ffn1_w_gate"], W["ffn1_w_up"], W["ffn1_w_down"], 0, store_a)
        if STOP == "A":
            return

        cst = st.enter_context(nc.sbuf_tensor("cst", [128, 36], F32))
        g08 = st.enter_context(nc.sbuf_tensor("g08", [128, 128], F32))
        neglam = st.enter_context(nc.sbuf_tensor("neglam", [128, 1], F32))
        iw_sb = st.enter_context(nc.sbuf_tensor("iw_sb", [128, 16, 16], F32))
        with ExitStack() as ps:
            rowt = ps.enter_context(nc.sbuf_tensor("rowt", [128, 128], F32))
            lrow = ps.enter_context(nc.sbuf_tensor("lrow", [1, 4, 64], F32))
            grow = ps.enter_context(nc.sbuf_tensor("grow", [1, 128], F32))
            pr = ps.enter_context(nc.sbuf_tensor("pr", [1, 2, 64], F32))
            ee = ps.enter_context(nc.sbuf_tensor("ee", [1, 4], F32))
            pc = ps.enter_context(nc.psum_tensor(U("pc"), [128, 512], F32))
            P.op("dve", lambda e: e.memset(rowt[:], 0.0), writes=["rowt"])
            P.op("sp", lambda e: e.dma_start(out=rowt[0:4, :], in_=W["dsa_kv_g"].rearrange("o (c p) -> (o c) p", p=128)),
                 reads=["rowt"], writes=["rowt_a"], dma="c1")
            P.op("sp", lambda e: e.dma_start(out=rowt[4:36, :], in_=W["b_gate"].rearrange("g (c p) -> (g c) p", p=128)),
                 reads=["rowt"], writes=["rowt_b"], dma="c2")
            P.op("sp", lambda e: e.dma_start(out=lrow[:], in_=W["diff_lambda"].rearrange("(o a) d -> o a d", o=1)), writes=["lrow"], dma="c3")
            P.op("sp", lambda e: e.dma_start(out=grow[:], in_=W["diff_subln_g"]), writes=["grow"], dma="c4")
            P.op("pe", lambda e: e.matmul(pc[:, 0:36], lhsT=rowt[:], rhs=ident32[:, 0:36], start=True, stop=True),
                 reads=["rowt", "rowt_a", "rowt_b", "ident32"], writes=["pc"])
            P.op("dve", lambda e: e.tensor_copy(out=cst[:], in_=pc[:, 0:36]), reads=["pc"], writes=["cst"])
            P.op("pe", lambda e: e.matmul(pc[:, 128:256], lhsT=ones32[0:1, :], rhs=grow[0:1, :], start=True, stop=True),
                 reads=["grow", "ones32"], writes=["pc"])
            P.op("dve", lambda e: e.tensor_scalar(out=g08[:], in0=pc[:, 128:256], scalar1=1.0 - LAMBDA_INIT, scalar2=None, op0=ALU.mult),
                 reads=["pc"], writes=["g08"])
            P.op("dve", lambda e: e.tensor_tensor(out=pr[:, 0, :], in0=lrow[:, 0, :], in1=lrow[:, 1, :], op=ALU.mult), reads=["lrow"], writes=["pr0"])
            P.op("dve", lambda e: e.tensor_tensor(out=pr[:, 1, :], in0=lrow[:, 2, :], in1=lrow[:, 3, :], op=ALU.mult), reads=["lrow"], writes=["pr1"])
            P.op("dve", lambda e: e.reduce_sum(out=ee[:, 0:2], in_=pr[:], axis=AX.X), reads=["pr0", "pr1"], writes=["ee"])
            P.op("act", lambda e: e.activation(out=ee[:, 0:2], in_=ee[:, 0:2], func=AF.Exp), reads=["ee"], writes=["ee"])
            P.op("dve", lambda e: e.tensor_tensor(out=ee[:, 2:3], in0=ee[:, 1:2], in1=ee[:, 0:1], op=ALU.subtract), reads=["ee"], writes=["ee2"])
            P.op("dve", lambda e: e.tensor_scalar(out=ee[:, 2:3], in0=ee[:, 2:3], scalar1=-LAMBDA_INIT, scalar2=None, op0=ALU.add), reads=["ee2"], writes=["ee2"])
            P.op("pe", lambda e: e.matmul(pc[:, 256:257], lhsT=ones32[0:1, :], rhs=ee[0:1, 2:3], start=True, stop=True),
                 reads=["ee2", "ones32"], writes=["pc"])
            P.op("dve", lambda e: e.tensor_copy(out=neglam[:], in_=pc[:, 256:257]), reads=["pc"], writes=["neglam"])
            P.emit()

        akT = dscr("akT", [1024, NTOK], BF16)
        avs = dscr("avs", [NTOK, 1024], BF16)
        khT = dscr("khT", [1024, NTOK], BF16)
        vhs = dscr("vhs", [NTOK, 1024], BF16)
        ikT = dscr("ikT", [64, NTOK], BF16)
        aqT = dscr("aqT", [1024, NQ], BF16)
        bqT = dscr("bqT", [1024, NQ], BF16)
        iqT = dscr("iqT", [1024, NQ], BF16)

        def linear_tm(ws, Wap, KC, f0, F, xTsub, xkey, nsub, psum, pskeys, evac, ncols=256):
            nb = len(psum)
            cnt = 0
            for g0 in range(0, F, ncols):
                nc_ = min(ncols, F - g0)
                view, wkey = ws.load(Wap, 0, KC, f0 + g0, nc_)
                for sub in range(nsub):
                    bk = cnt % nb
                    cnt += 1
                    for kc in range(KC):
                        P.op("pe", lambda e, kc=kc, sub=sub, bk=bk, view=view, nc_=nc_: e.matmul(
                            psum[bk][:, 0:nc_], lhsT=xTsub(kc, sub), rhs=view[:, kc, 0:nc_], start=(kc == 0), stop=(kc == KC - 1)),
                            reads=[wkey, xkey(kc)], writes=[pskeys[bk]])
                    evac(sub, g0, nc_, psum[bk], pskeys[bk])

        def proj_phase():
            with ExitStack() as ps:
                sb = lambda name, shape, dt: ps.enter_context(nc.sbuf_tensor(U(name), shape, dt))
                xb = sb("xb", [128, DC, T], BF16)
                wsl = [sb(f"ws{i}", [128, 16 * 256], BF16) for i in range(4)]
                stg = [sb(f"stg{i}", [128, 8, T], BF16) for i in range(2)]
                stm = [sb(f"stm{i}", [128, 4, 1024], BF16) for i in range(2)]
                cT = sb("cT", [128, 4, T], F32)
                cnT = sb("cnT", [128, 4, T], BF16)
                sqt = sb("sqt", [128, T], F32)
                ssq = sb("ssq", [128, T], F32)
                rs = sb("rs", [128, T], F32)
                ikw = sb("ikw", [128, T], F32)
                ikb = sb("ikb", [64, T], BF16)
                psum = [ps.enter_context(nc.psum_tensor(U(f"ps{i}"), [128, 512], F32)) for i in range(8)]
                pskeys = [("ps", i) for i in range(8)]
                ws = WStream(wsl)
                P.op("dve", lambda e: e.memset(ikw[:], 0.0), writes=["ikw"])
                xk = lambda kc: ("xb", kc)
                nfm = {"n": 0}
                ntm = {"n": 0}

                def fm_proj(Wap, KC, f0, F, xT, xkey, dst, t0, scale=None, bank0=0):
                    s_ = nfm["n"] % 2
                    nfm["n"] += 1
                    stage = stg[s_]

                    def evac(j, pt, pk):
                        if scale is None:
                            P.op("act", lambda e: e.copy(out=stage[:, j, :], in_=pt[:, 0:T]), reads=[pk], writes=[("stg", s_, j)])
                        else:
                            P.op("act", lambda e: e.activation(out=stage[:, j, :], in_=pt[:, 0:T], func=AF.Copy, scale=scale),
                                 reads=[pk], writes=[("stg", s_, j)])
                    linear(ws, Wap, KC, f0, F, xT, xkey, T, psum[bank0:bank0 + 4], pskeys[bank0:bank0 + 4], evac)
                    nj = F // 128
                    P.op("sp", lambda e: e.dma_start(out=dst[:, t0:t0 + T].rearrange("(c p) t -> p c t", p=128), in_=stage[:, 0:nj, :]),
                         reads=[("stg", s_, j) for j in range(nj)], dma=f"stg{s_}")

                def tm_proj(Wap, KC, f0, F, xTsub, xkey, stage, s_, col0):
                    def evac(sub, g0, nc_, pt, pk):
                        P.op("dve", lambda e: e.tensor_copy(out=stage[:, sub, col0 + g0:col0 + g0 + nc_], in_=pt[:, 0:nc_]),
                             reads=[pk], writes=[("stm", s_, sub, col0 + g0)])
                    linear_tm(ws, Wap, KC, f0, F, xTsub, xkey, 4, psum[4:8], pskeys[4:8], evac)

                for tt in range(int(os.environ.get('KNTB', NTOK // T))):
                    t0 = tt * T
                    P.op("sp", lambda e, t0=t0: e.dma_start(out=xb[:], in_=x1T[:, t0:t0 + T].rearrange("(c p) t -> p c t", p=128)),
                         writes=[("xb", c) for c in range(DC)], dma="xb")
                    xT = lambda kc: xb[:, kc, :]
                    xTs = lambda kc, sub: xb[:, kc, sub * 128:(sub + 1) * 128]
                    fm_proj(W["w_in"], DC, 1024, 1024, xT, xk, akT, t0)
                    s_ = ntm["n"] % 2
                    ntm["n"] += 1
                    tm_proj(W["w_in"], DC, 2048, 1024, xTs, xk, stm[s_], s_, 0)
                    P.op("sp", lambda e, t0=t0, s_=s_: e.dma_start(out=avs[t0:t0 + T, :].rearrange("(s p) f -> p s f", p=128), in_=stm[s_][:]),
                         reads=[("stm", s_, sub, g) for sub in range(4) for g in range(0, 1024, 256)], dma=f"stm{s_}")
                    def evac_c(j, pt, pk):
                        P.op("act", lambda e: e.copy(out=cT[:, j, :], in_=pt[:, 0:T]), reads=[pk], writes=[("cT", j)])
                        P.op("act", lambda e: e.activation(out=sqt[:], in_=cT[:, j, :], func=AF.Square), reads=[("cT", j)], writes=["sqt"])
                        if j == 0:
                            P.op("dve", lambda e: e.tensor_copy(out=ssq[:], in_=sqt[:]), reads=["sqt"], writes=["ssq"])
                        else:
                            P.op("dve", lambda e: e.tensor_tensor(out=ssq[:], in0=ssq[:], in1=sqt[:], op=ALU.add), reads=["sqt", "ssq"], writes=["ssq"])
                    linear(ws, W["w_in"], DC, 4096, 512, xT, xk, T, psum[0:4], pskeys[0:4], evac_c)
                    P.op("pe", lambda e: e.matmul(psum[0][:, :], lhsT=ones32[:], rhs=ssq[:], start=True, stop=True),
                         reads=["ones32", "ssq"], writes=[pskeys[0]])
                    P.op("dve", lambda e: e.tensor_scalar(out=rs[:], in0=psum[0][:, :], scalar1=1.0 / 512, scalar2=LN_EPS, op0=ALU.mult, op1=ALU.add),
                         reads=[pskeys[0]], writes=["rs"])
                    P.op("act", lambda e: e.activation(out=rs[:], in_=rs[:], func=AF.Sqrt), reads=["rs"], writes=["rs"])
                    P.op("dve", lambda e: e.reciprocal(out=rs[:], in_=rs[:]), reads=["rs"], writes=["rs"])
                    for j in range(4):
                        P.op("dve", lambda e, j=j: e.scalar_tensor_tensor(out=cnT[:, j, :], in0=cT[:, j, :], scalar=cst[:, j:j + 1], in1=rs[:],
                                                                          op0=ALU.mult, op1=ALU.mult),
                             reads=[("cT", j), "rs", "cst"], writes=[("cn", j)])
                    cx = lambda kc: cnT[:, kc, :]
                    cxs = lambda kc, sub: cnT[:, kc, sub * 128:(sub + 1) * 128]
                    ck = lambda kc: ("cn", kc)
                    s1_ = nfm["n"] % 2
                    nfm["n"] += 1
                    s2_ = ntm["n"] % 2
                    ntm["n"] += 1
                    for h in range(8):
                        def evac_k(j, pt, pk, h=h, s1_=s1_):
                            P.op("act", lambda e: e.copy(out=stg[s1_][:, h, :], in_=pt[:, 0:T]), reads=[pk], writes=[("stg", s1_, h)])
                        linear(ws, W["dsa_w_uk"][h], 4, 0, 128, cx, ck, T, psum[(h % 4):(h % 4) + 1], pskeys[(h % 4):(h % 4) + 1], evac_k, ncols=128)
                        tm_proj(W["dsa_w_uv"][h], 4, 0, 128, cxs, ck, stm[s2_], s2_, h * 128)
                    P.op("sp", lambda e, t0=t0, s1_=s1_: e.dma_start(out=khT[:, t0:t0 + T].rearrange("(c p) t -> p c t", p=128), in_=stg[s1_][:]),
                         reads=[("stg", s1_, j) for j in range(8)], dma=f"stg{s1_}")
                    P.op("sp", lambda e, t0=t0, s2_=s2_: e.dma_start(out=vhs[t0:t0 + T, :].rearrange("(s p) f -> p s f", p=128), in_=stm[s2_][:]),
                         reads=[("stm", s2_, sub, h * 128) for sub in range(4) for h in range(8)], dma=f"stm{s2_}")
                    def evac_i(j, pt, pk):
                        P.op("act", lambda e: e.copy(out=ikw[0:80, :], in_=pt[0:80, 0:T]), reads=[pk, "ikw"], writes=["ikw"])
                    linear(ws, W["w_in"], DC, 5632, 80, xT, xk, T, psum[0:1], pskeys[0:1], evac_i, ncols=128)
                    P.op("dve", lambda e: e.tensor_copy(out=ikb[:], in_=ikw[0:64, :]), reads=["ikw"], writes=["ikb"])
                    P.op("sp", lambda e, t0=t0: e.dma_start(out=ikT[:, t0:t0 + T], in_=ikb[:]), reads=["ikb"], dma="ikb")
                    if tt < NQ // T:
                        for sub in range(4):
                            P.op("pe", lambda e, sub=sub: e.matmul(psum[1][:, 0:16], lhsT=ikw[:, sub * 128:(sub + 1) * 128], rhs=ident32[:, 64:80],
                                                                   start=True, stop=True), reads=["ikw", "ident32"], writes=[pskeys[1]])
                            P.op("dve", lambda e, sub=sub, tt=tt: e.tensor_copy(out=iw_sb[:, tt * 4 + sub, :], in_=psum[1][:, 0:16]),
                                 reads=[pskeys[1]], writes=[("iw", tt * 4 + sub)])
                        fm_proj(W["w_in"], DC, 0, 1024, xT, xk, aqT, t0, scale=64 ** -0.5)
                        fm_proj(W["w_in"], DC, 3072, 1024, xT, xk, bqT, t0, scale=128 ** -0.5)
                        fm_proj(W["w_in"], DC, 4608, 1024, xT, xk, iqT, t0)
                P.emit()

        proj_phase()
        if STOP == "B":
            return

        yaT = dscr("yaT", [1024, NQ], BF16)
        ybT = dscr("ybT", [1024, NQ], BF16)
        identb = st.enter_context(nc.sbuf_tensor("identb", [128, 128], BF16))
        btab = st.enter_context(nc.sbuf_tensor("btab_sb", [128, 8, 32], F32))
        bdiag = st.enter_context(nc.sbuf_tensor("bdiag_sb", [128, 8, 128], F32))
        mdiag = st.enter_context(nc.sbuf_tensor("mdiag_sb", [128, 128], F32))
        parc = st.enter_context(nc.sbuf_tensor("parc", [128, 1], F32))
        P.op("dve", lambda e: e.tensor_copy(out=identb[:], in_=ident32[:]), writes=["identb"])
        P.op("sp", lambda e: e.dma_start(out=btab[:].rearrange("p a b -> p (a b)"), in_=btab_d), writes=["btab"], dma="t1")
        P.op("sp", lambda e: e.dma_start(out=bdiag[:].rearrange("p a b -> p (a b)"), in_=bdiag_d), writes=["bdiag"], dma="t2")
        P.op("sp", lambda e: e.dma_start(out=mdiag[:], in_=mdiag_d), writes=["mdiag"], dma="t3")
        P.op("sp", lambda e: e.dma_start(out=parc[:], in_=par), writes=["parc"], dma="t4")
        negr = st.enter_context(nc.sbuf_tensor("negr_sb", [128, 256], F32))
        dtab = st.enter_context(nc.sbuf_tensor("dtab_sb", [128, 32], F32))
        P.op("sp", lambda e: e.dma_start(out=negr[:], in_=negr_d), writes=["negr"], dma="t5")
        P.op("sp", lambda e: e.dma_start(out=dtab[:], in_=dtab_d), writes=["dtab"], dma="t6")
        P.emit()
        NH = int(os.environ.get("KNH", 8))
        NQB = int(os.environ.get("KNQB", 16))

        def attn_heads(ps, ncomp, kT_d, v_d, qT_d, yT_d, maskT=None, DMt=None):
            sb = lambda name, shape, dt: ps.enter_context(nc.sbuf_tensor(U(name), shape, dt))
            KD = 128 // ncomp
            Wd = ncomp * 128
            kt = sb("kt", [128, NTOK], BF16)
            vt = sb("vt", [128, 32, 130], BF16)
            qz = [sb(f"qz{i}", [128, NQ], BF16) for i in range(ncomp)]
            pT = [sb(f"pT{i}", [128, 256], BF16) for i in range(2)]
            tmpd = sb("tmpd", [128, 256], F32)
            o32 = sb("o32", [128, 128], F32)
            o1 = sb("o1", [128, 128], F32)
            sqj = sb("sqj", [128, 128], F32)
            ysb = sb("ysb", [128, 128], BF16)
            yT = sb("yT", [128, NQ], BF16)
            small = sb("small", [128, 8], F32)
            ps_s = [ps.enter_context(nc.psum_tensor(U("pss"), [128, 512], F32)) for _ in range(2)]
            ps_o = [ps.enter_context(nc.psum_tensor(U("pso"), [128, 512], F32)) for _ in range(2)]
            pt = ps.enter_context(nc.psum_tensor(U("ptb"), [128, 256], BF16))
            P.op("dve", lambda e: e.memset(vt[:], 1.0), writes=["vt"])
            P.op("dve", lambda e: e.memset(yT[:], 0.0), writes=["yT"])
            cnt = 0
            for h in range(NH):
                P.op("sp", lambda e, h=h: e.dma_start(out=kt[:], in_=kT_d[h * 128:(h + 1) * 128, :]), writes=["kt"], dma="kt")
                for q4 in range(4):
                    P.op("sp", lambda e, h=h, q4=q4: e.dma_start(
                        out=vt[:, q4 * 8:(q4 + 1) * 8, 0:128],
                        in_=v_d[q4 * 1024:(q4 + 1) * 1024, h * 128:(h + 1) * 128].rearrange("(n p) e -> p n e", p=128)),
                        reads=["vt"], writes=[("vtq", q4)], dma=f"vt{q4}")
                for c in range(ncomp):
                    P.op("sp", lambda e, h=h, c=c: e.dma_start(out=qz[c][:], in_=qT_d[h * 128:(h + 1) * 128, :]), writes=[("qz", c)], dma=f"qz{c}")
                    if ncomp == 2:
                        lo_, hi_ = (64, 128) if c == 0 else (0, 64)
                        P.op("dve", lambda e, c=c, lo_=lo_, hi_=hi_: e.memset(qz[c][lo_:hi_, :], 0.0), reads=[("qz", c)], writes=[("qz", c)])
                for i in range(NQB):
                    tiles = [("m", j) for j in range(i + 1)] + [("o", j) for j in range(i + 1)]
                    for idx, (kind, j) in enumerate(tiles):
                        n = j if kind == "m" else 16 + j
                        slot = cnt % 2
                        cnt += 1
                        S = ps_s[slot]
                        skey = ("pss", slot)
                        for c in range(ncomp):
                            P.op("pe", lambda e, c=c, n=n, i=i, S=S: e.matmul(
                                S[:, c * 128:(c + 1) * 128], lhsT=kt[:, n * 128:(n + 1) * 128],
                                rhs=qz[c][:, i * 128:(i + 1) * 128], start=True, stop=True),
                                reads=["kt", ("qz", c)], writes=[skey])
                        pk = ("pT", slot)
                        diag = (kind == "m" and j == i)
                        d = i - j
                        col = d if kind == "m" else 16 + d
                        if maskT is None:
                            if diag:
                                for c in range(ncomp):
                                    P.op("dve", lambda e, c=c, S=S, h=h: e.tensor_tensor(out=tmpd[:, c * 128:(c + 1) * 128], in0=S[:, c * 128:(c + 1) * 128],
                                                                                          in1=bdiag[:, h, :], op=ALU.add),
                                         reads=[skey, "bdiag"], writes=["tmpd"])
                                P.op("act", lambda e, slot=slot: e.activation(out=pT[slot][:, 0:Wd], in_=tmpd[:, 0:Wd], func=AF.Exp),
                                     reads=["tmpd"], writes=[pk])
                            else:
                                P.op("act", lambda e, slot=slot, S=S, h=h, col=col: e.activation(
                                    out=pT[slot][:, 0:Wd], in_=S[:, 0:Wd], func=AF.Exp, bias=btab[:, h, col:col + 1]),
                                    reads=[skey, "btab"], writes=[pk])
                        else:
                            mi = i * (i + 1) + idx
                            slope = 2.0 ** (-(h + 1))
                            P.op("dve", lambda e, S=S, i=i, slope=slope: e.scalar_tensor_tensor(out=tmpd[:, 0:128], in0=DMt[:, i, :], scalar=slope,
                                                                                               in1=S[:, 0:128], op0=ALU.mult, op1=ALU.add),
                                 reads=[skey, ("DMt", i)], writes=["tmpd"])
                            if diag:
                                P.op("dve", lambda e, h=h: e.tensor_tensor(out=tmpd[:, 0:128], in0=tmpd[:, 0:128], in1=bdiag[:, h, :], op=ALU.add),
                                     reads=["tmpd", "bdiag"], writes=["tmpd"])
                                P.op("act", lambda e: e.activation(out=tmpd[:, 0:128], in_=tmpd[:, 0:128], func=AF.Exp), reads=["tmpd"], writes=["tmpd"])
                            else:
                                P.op("act", lambda e, h=h, col=col: e.activation(out=tmpd[:, 0:128], in_=tmpd[:, 0:128], func=AF.Exp,
                                                                                 bias=btab[:, h, col:col + 1]), reads=["tmpd", "btab"], writes=["tmpd"])
                            P.op("dve", lambda e, slot=slot, mi=mi: e.tensor_tensor(out=pT[slot][:, 0:128], in0=tmpd[:, 0:128],
                                                                                    in1=maskT[:, mi, :], op=ALU.mult),
                                 reads=["tmpd", ("maskT", i)], writes=[pk])
                        for c in range(ncomp):
                            P.op("pe", lambda e, c=c, slot=slot, n=n, idx=idx, last=len(tiles) - 1: e.matmul(
                                ps_o[c][:, 0:129], lhsT=pT[slot][:, c * 128:(c + 1) * 128], rhs=vt[:, n, 0:129],
                                start=(idx == 0), stop=(idx == last)),
                                reads=[pk, "vt", ("vtq", n // 8)], writes=[("pso", c)])
                    P.op("dve", lambda e: e.reciprocal(out=small[:, 0:1], in_=ps_o[0][:, 128:129]), reads=[("pso", 0)], writes=["sm0"])
                    if ncomp == 2:
                        P.op("dve", lambda e: e.reciprocal(out=small[:, 1:2], in_=ps_o[1][:, 128:129]), reads=[("pso", 1)], writes=["sm1"])
                        P.op("dve", lambda e: e.tensor_scalar(out=o32[:], in0=ps_o[0][:, 0:128], scalar1=small[:, 0:1], scalar2=None, op0=ALU.mult),
                             reads=[("pso", 0), "sm0"], writes=["o32"])
                        P.op("dve", lambda e: e.tensor_scalar(out=o1[:], in0=ps_o[1][:, 0:128], scalar1=small[:, 1:2], scalar2=neglam[:, 0:1],
                                                              op0=ALU.mult, op1=ALU.mult),
                             reads=[("pso", 1), "sm1", "neglam"], writes=["o1"])
                        P.op("dve", lambda e: e.tensor_tensor(out=o32[:], in0=o32[:], in1=o1[:], op=ALU.add), reads=["o32", "o1"], writes=["o32"])
                        P.op("act", lambda e: e.activation(out=sqj[:], in_=o32[:], func=AF.Square, accum_out=small[:, 2:3]),
                             reads=["o32"], writes=["sqj", "sm2"])
                        P.op("dve", lambda e: e.tensor_scalar(out=small[:, 3:4], in0=small[:, 2:3], scalar1=1.0 / 128, scalar2=LN_EPS,
                                                              op0=ALU.mult, op1=ALU.add), reads=["sm2"], writes=["sm3"])
                        P.op("act", lambda e: e.activation(out=small[:, 3:4], in_=small[:, 3:4], func=AF.Sqrt), reads=["sm3"], writes=["sm3"])
                        P.op("dve", lambda e: e.reciprocal(out=small[:, 3:4], in_=small[:, 3:4]), reads=["sm3"], writes=["sm3"])
                        P.op("dve", lambda e: e.scalar_tensor_tensor(out=ysb[:], in0=o32[:], scalar=small[:, 3:4], in1=g08[:],
                                                                     op0=ALU.mult, op1=ALU.mult), reads=["o32", "sm3", "g08"], writes=["ysb"])
                    else:
                        P.op("dve", lambda e: e.tensor_scalar(out=ysb[:], in0=ps_o[0][:, 0:128], scalar1=small[:, 0:1], scalar2=None, op0=ALU.mult),
                             reads=[("pso", 0), "sm0"], writes=["ysb"])
                    P.op("pe", lambda e: e.transpose(pt[:, 0:128], ysb[:], identb[:]), reads=["ysb", "identb"], writes=["ptb"])
                    P.op("act", lambda e, i=i: e.copy(out=yT[:, i * 128:(i + 1) * 128], in_=pt[:, 0:128]), reads=["ptb"], writes=["yT"])
                P.op("sp", lambda e, h=h: e.dma_start(out=yT_d[h * 128:(h + 1) * 128, :], in_=yT[:]), reads=["yT"], dma="yT")

        with ExitStack() as ps:
            attn_heads(ps, 2, akT, avs, aqT, yaT)
            P.emit()
        if STOP == "D":
            return

        with ExitStack() as ps:
            sb = lambda name, shape, dt: ps.enter_context(nc.sbuf_tensor(U(name), shape, dt))
            maskT = sb("maskT", [128, 272, 128], BF16)
            DMt = sb("DMt", [128, 16, 128], F32)
            with ExitStack() as ps2:
                sb2 = lambda name, shape, dt: ps2.enter_context(nc.sbuf_tensor(U(name), shape, dt))
                iq = sb2("iq", [128, 8, NQ], BF16)
                ikz = [sb2(f"ikz{i}", [128, NTOK], BF16) for i in range(2)]
                acc = sb2("acc", [128, NTOK], F32)
                rr = [sb2(f"rr{i}", [128, 512], F32) for i in range(2)]
                msk = sb2("msk", [128, NTOK], BF16)
                junk = sb2("junk", [128, NQ], BF16)
                sm = sb2("sm", [128, 16], F32)
                cand = sb2("cand", [128, 128], F32)
                cmx = sb2("cmx", [128, 32], F32)
                dmb = sb2("dmb", [128, 128], F32)
                psi = [ps2.enter_context(nc.psum_tensor(U("psi"), [128, 512], F32)) for _ in range(2)]
                pti = ps2.enter_context(nc.psum_tensor(U("pti"), [128, 256], BF16))
                P.op("sp", lambda e: e.dma_start(out=iq[:], in_=iqT.rearrange("(c p) t -> p c t", p=128)), writes=["iq"], dma="iq")
                P.op("dve", lambda e: e.memset(ikz[0][:], 0.0), writes=["ikd0"])
                P.op("dve", lambda e: e.memset(ikz[1][:], 0.0), writes=["ikd1"])
                P.op("sp", lambda e: e.dma_start(out=ikz[0][0:64, :], in_=ikT), reads=["ikd0"], writes=["ikd0b"], dma="ik0")
                P.op("sp", lambda e: e.dma_start(out=ikz[1][64:128, :], in_=ikT), reads=["ikd1"], writes=["ikd1b"], dma="ik1")
                cnt = 0
                for i in range(NQB):
                    w_ = (i + 1) * 128
                    pieces = []
                    for base in (0, 2048):
                        for c0 in range(0, w_, 512):
                            pieces.append((base + c0, min(512, w_ - c0)))
                    for (c0, pw) in pieces:
                        for hh in range(16):
                            hp, half = hh // 2, hh % 2
                            slot = cnt % 2
                            cnt += 1
                            P.op("pe", lambda e, slot=slot, hp=hp, half=half, i=i, c0=c0, pw=pw: e.matmul(
                                psi[slot][:, 0:pw], lhsT=iq[:, hp, i * 128:(i + 1) * 128],
                                rhs=ikz[half][:, c0:c0 + pw], start=True, stop=True),
                                reads=["iq", "ikd0", "ikd1", "ikd0b", "ikd1b"], writes=[("psi", slot)])
                            P.op("act", lambda e, slot=slot, pw=pw: e.activation(out=rr[slot][:, 0:pw], in_=psi[slot][:, 0:pw], func=AF.Relu),
                                 reads=[("psi", slot)], writes=[("rr", slot)])
                            if hh == 0:
                                P.op("dve", lambda e, slot=slot, c0=c0, pw=pw, i=i: e.tensor_scalar(
                                    out=acc[:, c0:c0 + pw], in0=rr[slot][:, 0:pw], scalar1=iw_sb[:, i, 0:1], scalar2=None, op0=ALU.mult),
                                    reads=[("rr", slot), ("iw", i)], writes=["acc"])
                            else:
                                P.op("dve", lambda e, slot=slot, c0=c0, pw=pw, i=i, hh=hh: e.scalar_tensor_tensor(
                                    out=acc[:, c0:c0 + pw], in0=rr[slot][:, 0:pw], scalar=iw_sb[:, i, hh:hh + 1], in1=acc[:, c0:c0 + pw],
                                    op0=ALU.mult, op1=ALU.add), reads=[("rr", slot), ("iw", i), "acc"], writes=["acc"])
                    mr = (0, w_)
                    orr = (2048, 2048 + w_)
                    P.op("dve", lambda e, i=i: e.tensor_tensor(out=acc[:, i * 128:(i + 1) * 128], in0=acc[:, i * 128:(i + 1) * 128], in1=mdiag[:], op=ALU.add),
                         reads=["acc", "mdiag"], writes=["acc"])
                    P.op("dve", lambda e, i=i: e.tensor_scalar(out=acc[:, 2048 + i * 128:2048 + (i + 1) * 128], in0=acc[:, 2048 + i * 128:2048 + (i + 1) * 128],
                                                               scalar1=parc[:, 0:1], scalar2=None, op0=ALU.add),
                         reads=["acc", "parc"], writes=["acc"])
                    P.op("dve", lambda e, mr=mr: e.reduce_max(out=sm[:, 0:1], in_=acc[:, mr[0]:mr[1]], axis=AX.X), reads=["acc"], writes=["sm"])
                    P.op("dve", lambda e, orr=orr: e.reduce_max(out=sm[:, 2:3], in_=acc[:, orr[0]:orr[1]], axis=AX.X), reads=["acc", "sm"], writes=["sm"])
                    P.op("dve", lambda e: e.tensor_tensor(out=sm[:, 0:1], in0=sm[:, 0:1], in1=sm[:, 2:3], op=ALU.max), reads=["sm"], writes=["sm"])
                    P.op("dve", lambda e: e.memset(sm[:, 1:2], -1000.0), reads=["sm"], writes=["sm"])
                    for it in range(24):
                        P.op("dve", lambda e: e.tensor_tensor(out=sm[:, 2:3], in0=sm[:, 0:1], in1=sm[:, 1:2], op=ALU.add), reads=["sm"], writes=["sm"])
                        P.op("dve", lambda e: e.tensor_scalar(out=sm[:, 2:3], in0=sm[:, 2:3], scalar1=0.5, scalar2=None, op0=ALU.mult), reads=["sm"], writes=["sm"])
                        P.op("dve", lambda e, mr=mr, w_=w_: e.tensor_scalar(out=junk[:, 0:w_], in0=acc[:, mr[0]:mr[1]], scalar1=sm[:, 2:3], scalar2=None,
                                                              op0=ALU.is_ge, op1=ALU.add, accum_out=sm[:, 3:4]), reads=["acc", "sm"], writes=["junk", "sm"])
                        P.op("dve", lambda e, orr=orr, w_=w_: e.tensor_scalar(out=junk[:, 0:w_], in0=acc[:, orr[0]:orr[1]], scalar1=sm[:, 2:3], scalar2=None,
                                                              op0=ALU.is_ge, op1=ALU.add, accum_out=sm[:, 4:5]), reads=["acc", "sm"], writes=["junk", "sm"])
                        P.op("dve", lambda e: e.tensor_tensor(out=sm[:, 3:4], in0=sm[:, 3:4], in1=sm[:, 4:5], op=ALU.add), reads=["sm"], writes=["sm"])
                        P.op("dve", lambda e: e.tensor_scalar(out=sm[:, 5:6], in0=sm[:, 3:4], scalar1=255.5, scalar2=None, op0=ALU.is_ge), reads=["sm"], writes=["sm"])
                        P.op("dve", lambda e: e.tensor_tensor(out=sm[:, 6:7], in0=sm[:, 2:3], in1=sm[:, 1:2], op=ALU.subtract), reads=["sm"], writes=["sm"])
                        P.op("dve", lambda e: e.scalar_tensor_tensor(out=sm[:, 1:2], in0=sm[:, 6:7], scalar=sm[:, 5:6], in1=sm[:, 1:2], op0=ALU.mult, op1=ALU.add),
                             reads=["sm"], writes=["sm"])
                        P.op("dve", lambda e: e.tensor_tensor(out=sm[:, 6:7], in0=sm[:, 0:1], in1=sm[:, 2:3], op=ALU.subtract), reads=["sm"], writes=["sm"])
                        P.op("dve", lambda e: e.scalar_tensor_tensor(out=sm[:, 0:1], in0=sm[:, 6:7], scalar=sm[:, 5:6], in1=sm[:, 2:3], op0=ALU.mult, op1=ALU.add),
                             reads=["sm"], writes=["sm"])
                    for (a_, b_) in (mr, orr):
                        P.op("dve", lambda e, a_=a_, b_=b_: e.tensor_scalar(out=msk[:, a_:b_], in0=acc[:, a_:b_], scalar1=sm[:, 1:2], scalar2=None, op0=ALU.is_ge),
                             reads=["acc", "sm"], writes=["msk"])
                    tiles = [j for j in range(i + 1)] + [16 + j for j in range(i + 1)]
                    BIGC = 8192.0
                    for idx, n in enumerate(tiles):
                        j = n % 16
                        if n < 16 and j == i:
                            P.op("dve", lambda e, n=n: e.scalar_tensor_tensor(out=cand[:], in0=negr[:, 128:256], scalar=BIGC, in1=msk[:, n * 128:(n + 1) * 128],
                                                                              op0=ALU.add, op1=ALU.mult), reads=["negr", "msk"], writes=["cand"])
                        else:
                            col = (i - j) if n < 16 else 16 + (i - j)
                            P.op("dve", lambda e, n=n, col=col: e.scalar_tensor_tensor(out=cand[:], in0=negr[:, 0:128], scalar=dtab[:, col:col + 1],
                                                                                       in1=msk[:, n * 128:(n + 1) * 128], op0=ALU.add, op1=ALU.mult),
                                 reads=["negr", "dtab", "msk"], writes=["cand"])
                        P.op("dve", lambda e, idx=idx: e.reduce_max(out=cmx[:, idx:idx + 1], in_=cand[:], axis=AX.X), reads=["cand"], writes=["cmx"])
                    nt_ = len(tiles)
                    P.op("dve", lambda e, nt_=nt_: e.reduce_max(out=sm[:, 8:9], in_=cmx[:, 0:nt_], axis=AX.X), reads=["cmx", "sm"], writes=["sm"])
                    P.op("dve", lambda e: e.tensor_scalar(out=sm[:, 8:9], in0=sm[:, 8:9], scalar1=-1.0, scalar2=BIGC, op0=ALU.mult, op1=ALU.add),
                         reads=["sm"], writes=["sm"])
                    P.op("dve", lambda e: e.tensor_scalar(out=dmb[:], in0=ones32[:], scalar1=sm[:, 8:9], scalar2=None, op0=ALU.mult),
                         reads=["sm", "ones32"], writes=["dmb"])
                    P.op("pe", lambda e: e.matmul(psi[0][:, 0:128], lhsT=dmb[:], rhs=ident32[:], start=True, stop=True),
                         reads=["dmb", "ident32"], writes=[("psi", 0)])
                    P.op("act", lambda e, i=i: e.copy(out=DMt[:, i, :], in_=psi[0][:, 0:128]), reads=[("psi", 0)], writes=[("DMt", i)])
                    for idx, n in enumerate(tiles):
                        half = idx % 2
                        P.op("pe", lambda e, n=n, half=half: e.transpose(pti[:, half * 128:(half + 1) * 128], msk[:, n * 128:(n + 1) * 128], identb[:]),
                             reads=["msk", "identb"], writes=["pti"])
                        mi = i * (i + 1) + idx
                        P.op("act", lambda e, mi=mi, half=half: e.copy(out=maskT[:, mi, :], in_=pti[:, half * 128:(half + 1) * 128]),
                             reads=["pti"], writes=[("maskT", i)])
                P.emit()
            if STOP == "E":
                return
            with ExitStack() as ps3:
                attn_heads(ps3, 1, khT, vhs, bqT, ybT, maskT=maskT, DMt=DMt)
                P.emit()
        if STOP == "F":
            return

        x2T = dscr("x2T", [D, NQ], BF16)
        x2f = dscr("x2f", [D, NQ], F32)
        x3T = dscr("x3T", [D, NQ], BF16)
        x3f = dscr("x3f", [D, NQ], F32)
        onesb = st.enter_context(nc.sbuf_tensor("onesb", [128, 128], BF16))
        P.op("dve", lambda e: e.tensor_copy(out=onesb[:], in_=ones32[:]), writes=["onesb"])
        NTG = int(os.environ.get("KNTG", NQ // T))

        def make_evac_resid(z, zname, s1, s2, sq, coef):
            def evac(j, pt, pk):
                P.op("dve", lambda e: e.scalar_tensor_tensor(out=z[:, j, :], in0=pt[:, 0:T], scalar=coef, in1=z[:, j, :], op0=ALU.mult, op1=ALU.add),
                     reads=[pk, (zname, j)], writes=[(zname, j)])
                q = sq[j % 2]
                P.op("act", lambda e: e.activation(out=q[:], in_=z[:, j, :], func=AF.Square), reads=[(zname, j)], writes=[("sq", j % 2)])
                if j == 0:
                    P.op("dve", lambda e: e.tensor_copy(out=s1[:], in_=z[:, j, :]), reads=[(zname, j)], writes=["s1"])
                    P.op("dve", lambda e: e.tensor_copy(out=s2[:], in_=q[:]), reads=[("sq", j % 2)], writes=["s2"])
                else:
                    P.op("dve", lambda e: e.tensor_tensor(out=s1[:], in0=s1[:], in1=z[:, j, :], op=ALU.add), reads=[(zname, j), "s1"], writes=["s1"])
                    P.op("dve", lambda e: e.tensor_tensor(out=s2[:], in0=s2[:], in1=q[:], op=ALU.add), reads=[("sq", j % 2), "s2"], writes=["s2"])
            return evac

        def ln_bufs(sb):
            return dict(s1=sb("s1", [128, T], F32), s2=sb("s2", [128, T], F32), sq=[sb(f"sq{i}", [128, T], F32) for i in range(2)],
                        mean=sb("mean", [128, T], F32), rstd=sb("rstd", [128, T], F32), t2=sb("t2", [128, T], F32))

        with ExitStack() as ps:
            sb = lambda name, shape, dt: ps.enter_context(nc.sbuf_tensor(U(name), shape, dt))
            xb = sb("xb", [128, DC, T], BF16)
            ya = sb("ya", [128, 8, T], BF16)
            yb = sb("yb", [128, 8, T], BF16)
            mb = sb("mb", [128, DC, T], BF16)
            z = sb("z", [128, DC, T], F32)
            ob = sb("ob", [128, DC, T], BF16)
            wsl = [sb(f"ws{i}", [128, 16 * 256], BF16) for i in range(4)]
            sga = [sb(f"sga{i}", [128, T], F32) for i in range(2)]
            sgb = [sb(f"sgb{i}", [128, T], F32) for i in range(2)]
            tm = [sb(f"tm{i}", [128, T], F32) for i in range(2)]
            tmp = sb("tmp", [128, T], F32)
            L = ln_bufs(sb)
            psum = [ps.enter_context(nc.psum_tensor(U(f"ps{i}"), [128, 512], F32)) for i in range(8)]
            pskeys = [("ps", i) for i in range(8)]
            ws = WStream(wsl)
            for tt in range(NTG):
                t0 = tt * T
                fmv = lambda ap: ap[:, t0:t0 + T].rearrange("(c p) t -> p c t", p=128)
                P.op("sp", lambda e, v=fmv(x1T): e.dma_start(out=xb[:], in_=v), writes=[("xb", c) for c in range(DC)], dma="g_xb")
                P.op("sp", lambda e, v=fmv(yaT): e.dma_start(out=ya[:], in_=v), writes=[("ya", c) for c in range(8)], dma="g_ya")
                P.op("sp", lambda e, v=fmv(ybT): e.dma_start(out=yb[:], in_=v), writes=[("yb", c) for c in range(8)], dma="g_yb")
                P.op("sp", lambda e, v=fmv(x1f): e.dma_start(out=z[:], in_=v), writes=[("z", c) for c in range(DC)], dma="g_z")
                for g in range(8):
                    def ev_g0(j, pt, pk, g=g):
                        jg = g * 2 + j
                        P.op("act", lambda e: e.activation(out=sga[j][:], in_=pt[:, 0:T], func=AF.Sigmoid, bias=cst[:, 4 + jg:5 + jg]),
                             reads=[pk, "cst"], writes=[("sga", j)])

                    def ev_a(j, pt, pk):
                        P.op("dve", lambda e: e.tensor_tensor(out=tm[j][:], in0=pt[:, 0:T], in1=sga[j][:], op=ALU.mult),
                             reads=[pk, ("sga", j)], writes=[("tm", j)])

                    def ev_g1(j, pt, pk, g=g):
                        jg = g * 2 + j
                        P.op("act", lambda e: e.activation(out=sgb[j][:], in_=pt[:, 0:T], func=AF.Sigmoid, bias=cst[:, 20 + jg:21 + jg]),
                             reads=[pk, "cst"], writes=[("sgb", j)])

                    def ev_b(j, pt, pk, g=g):
                        jg = g * 2 + j
                        P.op("dve", lambda e: e.tensor_tensor(out=tmp[:], in0=pt[:, 0:T], in1=sgb[j][:], op=ALU.mult),
                             reads=[pk, ("sgb", j)], writes=["tmp"])
                        P.op("dve", lambda e: e.tensor_tensor(out=mb[:, jg, :], in0=tmp[:], in1=tm[j][:], op=ALU.add),
                             reads=["tmp", ("tm", j)], writes=[("mb", jg)])
                    linear(ws, W["w_gate"][0], DC, g * 256, 256, lambda kc: xb[:, kc, :], lambda kc: ("xb", kc), T, psum[0:2], pskeys[0:2], ev_g0)
                    linear(ws, W["w_branch_a"], 8, g * 256, 256, lambda kc: ya[:, kc, :], lambda kc: ("ya", kc), T, psum[2:4], pskeys[2:4], ev_a)
                    linear(ws, W["w_gate"][1], DC, g * 256, 256, lambda kc: xb[:, kc, :], lambda kc: ("xb", kc), T, psum[4:6], pskeys[4:6], ev_g1)
                    linear(ws, W["w_branch_b"], 8, g * 256, 256, lambda kc: yb[:, kc, :], lambda kc: ("yb", kc), T, psum[6:8], pskeys[6:8], ev_b)
                linear(ws, W["w_mix_out"], DC, 0, D, lambda kc: mb[:, kc, :], lambda kc: ("mb", kc), T, psum[0:4], pskeys[0:4],
                       make_evac_resid(z, "z", L["s1"], L["s2"], L["sq"], 1.0 / ALPHA))
                layer_norm(z, lambda c: ("z", c), L["s1"], L["s2"], psum[4], pskeys[4], psum[5], pskeys[5], (L["mean"], L["rstd"], L["t2"]), 1,
                           [(lambda c: ob[:, c, :], lambda c: ("ob", c)), (lambda c: z[:, c, :], lambda c: ("z", c))])
                P.op("sp", lambda e, v=fmv(x2T): e.dma_start(out=v, in_=ob[:]), reads=[("ob", c) for c in range(DC)], dma="g_so")
                P.op("sp", lambda e, v=fmv(x2f): e.dma_start(out=v, in_=z[:]), reads=[("z", c) for c in range(DC)], dma="g_sz")
            P.emit()
        if STOP == "G":
            return

        with ExitStack() as ps:
            sb = lambda name, shape, dt: ps.enter_context(nc.sbuf_tensor(U(name), shape, dt))
            xb = sb("xb", [128, DC, T], BF16)
            z = sb("z", [128, DC, T], F32)
            ob = sb("ob", [128, DC, T], BF16)
            wsl = [sb(f"ws{i}", [128, 16 * 256], BF16) for i in range(4)]
            mtok = sb("mtok", [128, D], F32)
            memTb = sb("memTb", [128, DC, 256], BF16)
            kTm = sb("kTm", [128, 4, 256], BF16)
            vm = sb("vm", [128, 2, 512], BF16)
            qm = sb("qm", [128, 4, T], BF16)
            pm = [sb(f"pm{i}", [128, T], BF16) for i in range(2)]
            om = sb("om", [128, 4, T], BF16)
            rz = sb("rz", [128, T], F32)
            L = ln_bufs(sb)
            psum = [ps.enter_context(nc.psum_tensor(U(f"ps{i}"), [128, 512], F32)) for i in range(8)]
            pskeys = [("ps", i) for i in range(8)]
            ws = WStream(wsl)
            for sub in range(2):
                P.op("sp", lambda e, sub=sub: e.dma_start(out=mtok[:], in_=memb[sub * 128:(sub + 1) * 128, :]), writes=["mtok"], dma="h_m")
                for c in range(DC):
                    bk = c % 2
                    P.op("pe", lambda e, c=c, bk=bk: e.transpose(psum[bk][:, 0:128], mtok[:, c * 128:(c + 1) * 128], ident32[:]),
                         reads=["mtok", "ident32"], writes=[pskeys[bk]])
                    P.op("act", lambda e, c=c, bk=bk, sub=sub: e.copy(out=memTb[:, c, sub * 128:(sub + 1) * 128], in_=psum[bk][:, 0:128]),
                         reads=[pskeys[bk]], writes=[("memTb", c)])

            def ev_k(j, pt, pk):
                P.op("act", lambda e: e.copy(out=kTm[:, j, :], in_=pt[:, 0:256]), reads=[pk], writes=[("kTm", j)])
            linear(ws, W["mem_w_kv"], DC, 0, 512, lambda kc: memTb[:, kc, :], lambda kc: ("memTb", kc), 256, psum[0:4], pskeys[0:4], ev_k)

            def ev_v(sub, g0, nc_, pt, pk):
                P.op("dve", lambda e: e.tensor_copy(out=vm[:, sub, g0:g0 + nc_], in_=pt[:, 0:nc_]), reads=[pk], writes=[("vm", sub, g0)])
            linear_tm(ws, W["mem_w_kv"], DC, 512, 512, lambda kc, sub: memTb[:, kc, sub * 128:(sub + 1) * 128], lambda kc: ("memTb", kc),
                      2, psum[4:8], pskeys[4:8], ev_v)
            vmkeys = [("vm", sub, g0) for sub in range(2) for g0 in (0, 256)]
            for tt in range(NTG):
                t0 = tt * T
                fmv = lambda ap: ap[:, t0:t0 + T].rearrange("(c p) t -> p c t", p=128)
                P.op("sp", lambda e, v=fmv(x2T): e.dma_start(out=xb[:], in_=v), writes=[("xb", c) for c in range(DC)], dma="h_xb")
                P.op("sp", lambda e, v=fmv(x2f): e.dma_start(out=z[:], in_=v), writes=[("z", c) for c in range(DC)], dma="h_z")

                def ev_q(j, pt, pk):
                    P.op("act", lambda e: e.activation(out=qm[:, j, :], in_=pt[:, 0:T], func=AF.Copy, scale=128 ** -0.5), reads=[pk], writes=[("qm", j)])
                linear(ws, W["mem_w_q"], DC, 0, 512, lambda kc: xb[:, kc, :], lambda kc: ("xb", kc), T, psum[0:4], pskeys[0:4], ev_q)
                for h in range(4):
                    for s_ in range(2):
                        P.op("pe", lambda e, h=h, s_=s_: e.matmul(psum[4 + s_][:, 0:T], lhsT=kTm[:, h, s_ * 128:(s_ + 1) * 128], rhs=qm[:, h, :],
                                                                  start=True, stop=True), reads=[("kTm", h), ("qm", h)], writes=[pskeys[4 + s_]])
                        P.op("act", lambda e, s_=s_: e.activation(out=pm[s_][:], in_=psum[4 + s_][:, 0:T], func=AF.Exp),
                             reads=[pskeys[4 + s_]], writes=[("pm", s_)])
                    for s_ in range(2):
                        P.op("pe", lambda e, h=h, s_=s_: e.matmul(psum[6][:, 0:T], lhsT=vm[:, s_, h * 128:(h + 1) * 128], rhs=pm[s_][:],
                                                                  start=(s_ == 0), stop=(s_ == 1)), reads=vmkeys + [("pm", s_)], writes=[pskeys[6]])
                    for s_ in range(2):
                        P.op("pe", lambda e, s_=s_: e.matmul(psum[7][:, 0:T], lhsT=onesb[:], rhs=pm[s_][:], start=(s_ == 0), stop=(s_ == 1)),
                             reads=["onesb", ("pm", s_)], writes=[pskeys[7]])
                    P.op("dve", lambda e: e.reciprocal(out=rz[:], in_=psum[7][:, 0:T]), reads=[pskeys[7]], writes=["rz"])
                    P.op("dve", lambda e, h=h: e.tensor_tensor(out=om[:, h, :], in0=psum[6][:, 0:T], in1=rz[:], op=ALU.mult),
                         reads=[pskeys[6], "rz"], writes=[("om", h)])
                linear(ws, W["mem_w_o"], 4, 0, D, lambda kc: om[:, kc, :], lambda kc: ("om", kc), T, psum[0:4], pskeys[0:4],
                       make_evac_resid(z, "z", L["s1"], L["s2"], L["sq"], 1.0 / ALPHA))
                layer_norm(z, lambda c: ("z", c), L["s1"], L["s2"], psum[4], pskeys[4], psum[5], pskeys[5], (L["mean"], L["rstd"], L["t2"]), 2,
                           [(lambda c: ob[:, c, :], lambda c: ("ob", c)), (lambda c: z[:, c, :], lambda c: ("z", c))])
                P.op("sp", lambda e, v=fmv(x3T): e.dma_start(out=v, in_=ob[:]), reads=[("ob", c) for c in range(DC)], dma="h_so")
                P.op("sp", lambda e, v=fmv(x3f): e.dma_start(out=v, in_=z[:]), reads=[("z", c) for c in range(DC)], dma="h_sz")
            P.emit()
        if STOP == "H":
            return

        def load_x3(tt, env):
            t0 = tt * T
            fmv = lambda ap: ap[:, t0:t0 + T].rearrange("(c p) t -> p c t", p=128)
            P.op("sp", lambda e, v=fmv(x3f): e.dma_start(out=env["xT32"][:], in_=v), writes=[("xT32", c) for c in range(DC)], dma="i_x32")
            P.op("sp", lambda e, v=fmv(x3T): e.dma_start(out=env["xTb"][:], in_=v), writes=[("xTb", c) for c in range(DC)], dma="i_xb")

        def store_i(tt, env):
            xT32, xtok, psum, pskeys = env["xT32"], env["xtok"], env["psum"], env["pskeys"]
            t0 = tt * T

            def after():
                for sub in range(4):
                    ot = xtok[sub % 2]
                    for c in range(DC):
                        bk = 6 + (c % 2)
                        P.op("pe", lambda e, c=c, bk=bk, sub=sub: e.transpose(psum[bk][:, 0:128], xT32[:, c, sub * 128:(sub + 1) * 128], ident32[:]),
                             reads=[("xT32", c), "ident32"], writes=[pskeys[bk]])
                        P.op("act", lambda e, c=c, bk=bk, ot=ot: e.copy(out=ot[:, c * 128:(c + 1) * 128], in_=psum[bk][:, 0:128]),
                             reads=[pskeys[bk]], writes=[("xtok", sub % 2)])
                    r0 = t0 + sub * 128
                    P.op("sp", lambda e, r0=r0, ot=ot: e.dma_start(out=out[r0:r0 + 128, :], in_=ot[:]), reads=[("xtok", sub % 2)], dma=f"xtok{sub % 2}")
            return {"ln_outs": [(lambda c: xT32[:, c, :], lambda c: ("xT32", c))], "after": after}

        ffn_phase(NTG, load_x3, W["ffn2_w_gate"], W["ffn2_w_up"], W["ffn2_w_down"], 3, store_i)
    return


def build_nc(dbg=None):
    nc = bass.Bass("TRN2", target_bir_lowering=False)
    _build(nc, dbg)
    return nc


def _perm(p):
    mine = np.concatenate([np.arange(128) + 128 * (2 * i + p) for i in range(16)])
    other = np.concatenate([np.arange(128) + 128 * (2 * i + 1 - p) for i in range(16)])
    return mine, other


def make_in_maps(inputs):
    x = np.asarray(inputs["x"], dtype=np.float32)
    mem = np.asarray(inputs["mem"], dtype=np.float32)
    shared = {}
    for k, v in inputs.items():
        if k in ("x", "mem"):
            continue
        a = np.asarray(v, dtype=np.float32)
        a = a[0]
        if k in ("diff_subln_g", "dsa_kv_g"):
            a = a.reshape(1, -1)
        shared[k] = np.ascontiguousarray(a)
    maps = []
    for c in range(8):
        b, p = c // 2, c % 2
        mine, other = _perm(p)
        xlc = np.ascontiguousarray(x[b][np.concatenate([mine, other])])
        m = dict(shared)
        m["xl"] = xlc
        m["memb"] = np.ascontiguousarray(mem[b])
        m["par"] = np.full((128, 1), 0.0 if p == 1 else NEG, dtype=np.float32)
        srel = np.arange(128, dtype=np.float64)
        slopes = 2.0 ** (-(np.arange(8) + 1.0))
        bt = np.zeros((128, 8, 32), dtype=np.float64)
        for dd in range(16):
            bt[:, :, dd] = slopes[None, :] * (srel[:, None] - 127 - 256 * dd)
            delta = 2 * dd + (2 * p - 1)
            bt[:, :, 16 + dd] = slopes[None, :] * (srel[:, None] - 127 - 128 * delta) + (NEG if delta < 0 else 0.0)
        m["btab"] = np.ascontiguousarray(bt.reshape(128, 256).astype(np.float32))
        tt_, ss_ = np.meshgrid(np.arange(128), np.arange(128))
        adm = (ss_ // 64) <= (tt_ // 64)
        bd = np.where(adm[:, None, :], -slopes[None, :, None] * np.abs(tt_ - ss_)[:, None, :] + slopes[None, :, None] * (tt_[:, None, :] - 127), NEG)
        m["bdiag"] = np.ascontiguousarray(bd.reshape(128, 1024).astype(np.float32))
        m["mdiag"] = np.ascontiguousarray(np.where(adm.T, 0.0, NEG).astype(np.float32))
        tq, sk = np.meshgrid(np.arange(128), np.arange(128), indexing="ij")
        m["negr"] = np.ascontiguousarray(np.concatenate([(sk - tq), -np.abs(tq - sk)], axis=1).astype(np.float32))
        dt_ = np.zeros((128, 32), dtype=np.float32)
        for dd in range(16):
            dt_[:, dd] = 8192.0 - 128.0 * (2 * dd)
            dt_[:, 16 + dd] = 8192.0 - 128.0 * max(2 * dd + (2 * p - 1), 0)
        m["dtab"] = dt_
        maps.append(m)
    return maps


def kernel(**inputs):
    nc = build_nc()
    maps = make_in_maps(inputs)
    res = run_bass_kernel_spmd(nc, maps, core_ids=list(range(8)))
    outp = np.zeros((4, SEQ, D), dtype=np.float32)
    for c in range(8):
        b, p = c // 2, c % 2
        mine, _ = _perm(p)
        outp[b][mine] = res.results[c]["out"]
    return outp
```
